# Optimizing a Trainium2 kernel written in Bass

```python
import numpy as np
import jax
import jax.numpy as jnp
from jax import lax

D_MODEL = 1024
BATCH = 32
SEQ = 2048
DEPTH = 2

NSA_HEADS = 8
NSA_KV_HEADS = 2
NSA_HEAD_DIM = 64
NSA_GROUP = NSA_HEADS // NSA_KV_HEADS
NSA_WIDTH = NSA_HEADS * NSA_HEAD_DIM
NSA_KV_WIDTH = NSA_KV_HEADS * NSA_HEAD_DIM
CMP_BLOCK = 32
CMP_STRIDE = 16
CMP_HIDDEN = 256
SEL_BLOCK = 64
N_SELECT = 8
WINDOW = 512
QUERY_BLOCK = 64

GDN_HEADS = 4
GDN_HEAD_DIM = 128
GDN_WIDTH = GDN_HEADS * GDN_HEAD_DIM
GDN_CONV = 4
GDN_CHUNK = 64

MIX_WIDTH = NSA_WIDTH + GDN_WIDTH
D_FF = 2816
FFN_CONV = 3
EPS = 1e-6

SPLIT_SIZES = (NSA_WIDTH, 6 * NSA_KV_WIDTH, 3 * NSA_HEADS, 3 * GDN_WIDTH, GDN_WIDTH, GDN_HEADS, GDN_HEADS)
IN_WIDTH = sum(SPLIT_SIZES)
SPLIT_POINTS = tuple(int(v) for v in np.cumsum(SPLIT_SIZES)[:-1])

kernel_name = "hymba_nsa_gdn_convffn"


def rmsnorm(x, g):
    xf = x.astype(jnp.float32)
    y = xf * lax.rsqrt(jnp.mean(xf * xf, axis=-1, keepdims=True) + EPS)
    return (y * g.astype(jnp.float32)).astype(x.dtype)


def l2norm(x):
    xf = x.astype(jnp.float32)
    return xf * lax.rsqrt(jnp.sum(xf * xf, axis=-1, keepdims=True) + EPS)


def causal_dwconv(x, w):
    width = w.shape[0]
    s = x.shape[1]
    xp = jnp.pad(x, ((0, 0), (width - 1, 0), (0, 0)))
    y = xp[:, 0:s] * w[0]
    for k in range(1, width):
        y = y + xp[:, k:k + s] * w[k]
    return y


def masked_softmax(s, mask):
    s = jnp.where(mask, s.astype(jnp.float32), -jnp.inf)
    m = jnp.max(s, axis=-1, keepdims=True)
    m = jnp.where(jnp.isfinite(m), m, 0.0)
    p = jnp.where(mask, jnp.exp(s - m), 0.0)
    return p / jnp.maximum(jnp.sum(p, axis=-1, keepdims=True), 1e-30)


def compress(kv, pos, w1, b1, w2):
    b, s, h, dh = kv.shape
    n_cmp = (s - CMP_BLOCK) // CMP_STRIDE + 1
    idx = jnp.arange(n_cmp)[:, None] * CMP_STRIDE + jnp.arange(CMP_BLOCK)[None, :]
    blocks = kv[:, idx] + pos[None, None, :, None, :]
    flat = blocks.transpose(0, 3, 1, 2, 4).reshape(b, h, n_cmp, CMP_BLOCK * dh)
    return jax.nn.gelu(flat @ w1 + b1) @ w2


def nsa_mixer(q, kv, gates, pos_k, pos_v, ck_w1, ck_b1, ck_w2, cv_w1, cv_b1, cv_w2):
    b, s = q.shape[:2]
    h, g, dh = NSA_KV_HEADS, NSA_GROUP, NSA_HEAD_DIM
    kv = kv.reshape(b, s, 6, h, dh)
    k_cmp, v_cmp, k_slc, v_slc, k_win, v_win = (kv[:, :, i] for i in range(6))
    kc = compress(k_cmp, pos_k, ck_w1, ck_b1, ck_w2)
    vc = compress(v_cmp, pos_v, cv_w1, cv_b1, cv_w2)
    n_cmp = kc.shape[2]
    n_sb = s // SEL_BLOCK
    top = min(N_SELECT, n_sb)
    cmp_start = jnp.arange(n_cmp) * CMP_STRIDE
    sel_start = jnp.arange(n_sb) * SEL_BLOCK
    overlap = ((cmp_start[:, None] < sel_start[None, :] + SEL_BLOCK)
               & (sel_start[None, :] < cmp_start[:, None] + CMP_BLOCK)).astype(jnp.float32)
    ks_blk = k_slc.reshape(b, n_sb, SEL_BLOCK, h, dh).transpose(0, 3, 1, 2, 4)
    vs_blk = v_slc.reshape(b, n_sb, SEL_BLOCK, h, dh).transpose(0, 3, 1, 2, 4)
    kw_pad = jnp.pad(k_win, ((0, 0), (WINDOW, 0), (0, 0), (0, 0)))
    vw_pad = jnp.pad(v_win, ((0, 0), (WINDOW, 0), (0, 0), (0, 0)))
    qg = q.reshape(b, s, h, g, dh) * (NSA_HEAD_DIM ** -0.5)
    bi = jnp.arange(b)[:, None, None, None]
    hi = jnp.arange(h)[None, :, None, None]
    j_blk = jnp.arange(n_sb)

    def query_block(s0):
        t = s0 + jnp.arange(QUERY_BLOCK)
        qb = lax.dynamic_slice_in_dim(qg, s0, QUERY_BLOCK, axis=1)
        m_cmp = (cmp_start[None, :] + CMP_BLOCK - 1) <= t[:, None]
        p_cmp = masked_softmax(jnp.einsum('bqhgd,bhnd->bhgqn', qb, kc), m_cmp)
        o_cmp = jnp.einsum('bhgqn,bhnd->bqhgd', p_cmp, vc)
        imp = jnp.einsum('bhgqn,nj->bhqj', p_cmp, overlap)
        cur = t // SEL_BLOCK
        valid = sel_start[None, :] <= t[:, None]
        forced = (j_blk[None, :] == 0) | (j_blk[None, :] == cur[:, None]) | (j_blk[None, :] == cur[:, None] - 1)
        score = jnp.where(forced, jnp.inf, jnp.where(valid, imp, -jnp.inf))
        top_val, top_idx = lax.top_k(score, top)
        k_sel = ks_blk[bi, hi, top_idx].reshape(b, h, QUERY_BLOCK, top * SEL_BLOCK, dh)
        v_sel = vs_blk[bi, hi, top_idx].reshape(b, h, QUERY_BLOCK, top * SEL_BLOCK, dh)
        tok = (top_idx[..., None] * SEL_BLOCK + jnp.arange(SEL_BLOCK)).reshape(b, h, QUERY_BLOCK, top * SEL_BLOCK)
        m_sel = jnp.repeat(top_val > -jnp.inf, SEL_BLOCK, axis=-1) & (tok <= t[:, None])
        p_sel = masked_softmax(jnp.einsum('bqhgd,bhqnd->bhgqn', qb, k_sel), m_sel[:, :, None])
        o_sel = jnp.einsum('bhgqn,bhqnd->bqhgd', p_sel, v_sel)
        kw = lax.dynamic_slice_in_dim(kw_pad, s0, WINDOW + QUERY_BLOCK, axis=1)
        vw = lax.dynamic_slice_in_dim(vw_pad, s0, WINDOW + QUERY_BLOCK, axis=1)
        kpos = s0 - WINDOW + jnp.arange(WINDOW + QUERY_BLOCK)
        m_win = (kpos[None, :] >= 0) & (kpos[None, :] <= t[:, None]) & (kpos[None, :] > t[:, None] - WINDOW)
        p_win = masked_softmax(jnp.einsum('bqhgd,bkhd->bhgqk', qb, kw), m_win)
        o_win = jnp.einsum('bhgqk,bkhd->bqhgd', p_win, vw)
        return jnp.stack([o_cmp, o_sel, o_win], axis=-2)

    outs = lax.map(query_block, jnp.arange(s // QUERY_BLOCK) * QUERY_BLOCK)
    o = outs.transpose(1, 0, 2, 3, 4, 5, 6).reshape(b, s, NSA_HEADS, 3, dh)
    gate = jax.nn.sigmoid(gates.astype(jnp.float32).reshape(b, s, NSA_HEADS, 3))
    return jnp.einsum('bshc,bshcd->bshd', gate, o).reshape(b, s, NSA_WIDTH).astype(q.dtype)


def gated_delta_rule_chunked(q, k, v, g, beta):
    b, s, h, dk = q.shape
    dv = v.shape[-1]
    c = GDN_CHUNK
    n = s // c
    chunks = lambda a: a.reshape(b, n, c, h, -1).transpose(1, 0, 3, 2, 4)
    qc, kc, vc = chunks(q), chunks(k), chunks(v)
    gc = g.reshape(b, n, c, h).transpose(1, 0, 3, 2)
    bc = beta.reshape(b, n, c, h).transpose(1, 0, 3, 2)
    gcum = jnp.cumsum(gc, axis=-1)
    tril = jnp.tril(jnp.ones((c, c), dtype=bool))
    strict = jnp.tril(jnp.ones((c, c), dtype=bool), -1)
    decay = jnp.exp(jnp.where(tril, gcum[..., :, None] - gcum[..., None, :], -jnp.inf))
    kb = kc * bc[..., None]
    vb = vc * bc[..., None]
    a_mat = jnp.where(strict, jnp.einsum('nbhid,nbhjd->nbhij', kb, kc) * decay, 0.0)
    eye = jnp.eye(c, dtype=jnp.float32)
    rhs = jnp.concatenate([vb, kb * jnp.exp(gcum)[..., None]], axis=-1)
    sol = lax.linalg.triangular_solve(a_mat + eye, rhs, left_side=True, lower=True, unit_diagonal=True)
    u, w = sol[..., :dv], sol[..., dv:]
    attn = jnp.einsum('nbhid,nbhjd->nbhij', qc, kc) * decay

    def step(state, xs):
        q_i, k_i, u_i, w_i, g_i, a_i = xs
        v_new = u_i - jnp.einsum('bhck,bhkv->bhcv', w_i, state)
        o = (jnp.einsum('bhck,bhkv->bhcv', q_i * jnp.exp(g_i)[..., None], state)
             + jnp.einsum('bhcj,bhjv->bhcv', a_i, v_new))
        g_last = g_i[..., -1:]
        state = (state * jnp.exp(g_last)[..., None]
                 + jnp.einsum('bhck,bhcv->bhkv', k_i * jnp.exp(g_last - g_i)[..., None], v_new))
        return state, o

    state0 = jnp.zeros((b, h, dk, dv), jnp.float32)
    _, o = lax.scan(step, state0, (qc, kc, u, w, gcum, attn))
    return o.transpose(1, 0, 3, 2, 4).reshape(b, s, h, dv)


def gdn_mixer(qkv, z, b_in, a_in, conv_w, a_log, dt_bias, norm_g):
    b, s = qkv.shape[:2]
    h, d = GDN_HEADS, GDN_HEAD_DIM
    qkv = jax.nn.silu(causal_dwconv(qkv, conv_w))
    q, k, v = jnp.split(qkv, 3, axis=-1)
    q = l2norm(q.reshape(b, s, h, d)) * (d ** -0.5)
    k = l2norm(k.reshape(b, s, h, d))
    v = v.reshape(b, s, h, d).astype(jnp.float32)
    beta = jax.nn.sigmoid(b_in.astype(jnp.float32))
    g = -jnp.exp(a_log.astype(jnp.float32)) * jax.nn.softplus(a_in.astype(jnp.float32) + dt_bias.astype(jnp.float32))
    o = gated_delta_rule_chunked(q, k, v, g, beta)
    o = rmsnorm(o, norm_g) * jax.nn.silu(z.reshape(b, s, h, d).astype(jnp.float32))
    return o.reshape(b, s, GDN_WIDTH).astype(z.dtype)


def setup_inputs(seed: int = 0) -> dict:
    key = jax.random.key(seed)
    ks = jax.random.split(key, 24)
    L = DEPTH
    f32 = jnp.float32
    nrm = lambda k, shape, scale: jax.random.normal(k, shape, f32) * scale
    dt = jnp.exp(jax.random.uniform(ks[13], (L, GDN_HEADS), f32, np.log(1e-3), np.log(1e-1)))
    return {
        "x": nrm(ks[0], (BATCH, SEQ, D_MODEL), 1.0),
        "norm_mix": 1.0 + nrm(ks[1], (L, D_MODEL), 0.02),
        "w_in": nrm(ks[2], (L, D_MODEL, IN_WIDTH), D_MODEL ** -0.5),
        "cmp_pos_k": nrm(ks[3], (L, CMP_BLOCK, NSA_HEAD_DIM), 0.1),
        "cmp_pos_v": nrm(ks[4], (L, CMP_BLOCK, NSA_HEAD_DIM), 0.1),
        "cmp_k_w1": nrm(ks[5], (L, CMP_BLOCK * NSA_HEAD_DIM, CMP_HIDDEN), (CMP_BLOCK * NSA_HEAD_DIM) ** -0.5),
        "cmp_k_b1": nrm(ks[6], (L, CMP_HIDDEN), 0.01),
        "cmp_k_w2": nrm(ks[7], (L, CMP_HIDDEN, NSA_HEAD_DIM), CMP_HIDDEN ** -0.5),
        "cmp_v_w1": nrm(ks[8], (L, CMP_BLOCK * NSA_HEAD_DIM, CMP_HIDDEN), (CMP_BLOCK * NSA_HEAD_DIM) ** -0.5),
        "cmp_v_b1": nrm(ks[9], (L, CMP_HIDDEN), 0.01),
        "cmp_v_w2": nrm(ks[10], (L, CMP_HIDDEN, NSA_HEAD_DIM), CMP_HIDDEN ** -0.5),
        "nsa_norm": 1.0 + nrm(ks[11], (L, NSA_WIDTH), 0.02),
        "gdn_conv": nrm(ks[12], (L, GDN_CONV, 3 * GDN_WIDTH), GDN_CONV ** -0.5),
        "gdn_a_log": jnp.log(jax.random.uniform(ks[14], (L, GDN_HEADS), f32, 1.0, 16.0)),
        "gdn_dt_bias": dt + jnp.log(-jnp.expm1(-dt)),
        "gdn_norm": 1.0 + nrm(ks[15], (L, GDN_HEAD_DIM), 0.02),
        "w_out": nrm(ks[16], (L, MIX_WIDTH, D_MODEL), MIX_WIDTH ** -0.5),
        "norm_ffn": 1.0 + nrm(ks[17], (L, D_MODEL), 0.02),
        "ffn_up": nrm(ks[18], (L, D_MODEL, 2 * D_FF), D_MODEL ** -0.5),
        "ffn_conv": nrm(ks[19], (L, FFN_CONV, 2 * D_FF), FFN_CONV ** -0.5),
        "ffn_down": nrm(ks[20], (L, D_FF, D_MODEL), D_FF ** -0.5),
        "norm_final": 1.0 + nrm(ks[21], (D_MODEL,), 0.02),
    }


def reference(x, norm_mix, w_in, cmp_pos_k, cmp_pos_v, cmp_k_w1, cmp_k_b1, cmp_k_w2, cmp_v_w1, cmp_v_b1, cmp_v_w2,
              nsa_norm, gdn_conv, gdn_a_log, gdn_dt_bias, gdn_norm, w_out, norm_ffn, ffn_up, ffn_conv, ffn_down,
              norm_final):
    for l in range(DEPTH):
        h = rmsnorm(x, norm_mix[l])
        proj = h @ w_in[l]
        q_nsa, kv_nsa, gate_nsa, qkv_gdn, z_gdn, b_gdn, a_gdn = jnp.split(proj, SPLIT_POINTS, axis=-1)
        o_nsa = nsa_mixer(q_nsa, kv_nsa, gate_nsa, cmp_pos_k[l], cmp_pos_v[l],
                          cmp_k_w1[l], cmp_k_b1[l], cmp_k_w2[l], cmp_v_w1[l], cmp_v_b1[l], cmp_v_w2[l])
        o_nsa = rmsnorm(o_nsa, nsa_norm[l])
        o_gdn = gdn_mixer(qkv_gdn, z_gdn, b_gdn, a_gdn, gdn_conv[l], gdn_a_log[l], gdn_dt_bias[l], gdn_norm[l])
        x = x + jnp.concatenate([o_nsa, o_gdn], axis=-1) @ w_out[l]
        h = rmsnorm(x, norm_ffn[l])
        u = causal_dwconv(h @ ffn_up[l], ffn_conv[l])
        gate, up = jnp.split(u, 2, axis=-1)
        x = x + (jax.nn.silu(gate) * up) @ ffn_down[l]
    return rmsnorm(x, norm_final)
```

```python
import contextlib
import numpy as np
import ml_dtypes
import concourse.bass as bass
import concourse.mybir as mybir
from concourse.bass_utils import run_bass_kernel_spmd

F32 = mybir.dt.float32
BF16 = mybir.dt.bfloat16
AF = mybir.ActivationFunctionType
ALU = mybir.AluOpType
AX = mybir.AxisListType
DT = mybir.dt
_DSZ = {DT.float32: 4, DT.bfloat16: 2, DT.float16: 2, DT.int32: 4, DT.uint32: 4,
        DT.int16: 2, DT.uint16: 2, DT.int8: 1, DT.uint8: 1}

ENGS = ("pe", "act", "dve", "pool", "sp")
SAME_ENGINE_SYNC = True
DMA_POOL = 12

D_MODEL = 1024
IN_WIDTH = 3360
D_FF = 2816
EPS = 1e-6
NEG = -30000.0


def _box(ap):
    t = ap.tensor
    name = t.name
    esz = _DSZ[ap.dtype]
    dims = list(ap.ap)
    off = ap.offset
    if type(t).__name__.startswith("DRam"):
        lo = off
        hi = off
        for st, cnt in dims:
            if cnt > 1:
                hi += (cnt - 1) * abs(st)
        return (name, 0, 1, lo * esz, (hi + 1) * esz)
    pst, pcnt = dims[0]
    p_lo = off // pst if pst else 0
    p_hi = p_lo + pcnt
    if type(t).__name__.startswith("PSum"):
        return (name, (p_lo // 32) * 32, ((p_hi + 31) // 32) * 32, 0, 2048)
    f_lo = off - p_lo * pst if pst else off
    f_hi = f_lo
    for st, cnt in dims[1:]:
        if cnt > 1:
            f_hi += (cnt - 1) * abs(st)
    return (name, p_lo, p_hi, f_lo * esz, (f_hi + 1) * esz)


def _overlap(a, b):
    return a[1] < b[2] and b[1] < a[2] and a[3] < b[4] and b[3] < a[4]


def _covers(a, b):
    return a[1] <= b[1] and a[2] >= b[2] and a[3] <= b[3] and a[4] >= b[4]


class _Ins:
    __slots__ = ("eng", "fn", "deps", "signal", "dma", "idx")

    def __init__(self, eng, fn):
        self.eng = eng
        self.fn = fn
        self.deps = []
        self.signal = False
        self.dma = None
        self.idx = 0


class Sched:
    def __init__(self, nc):
        self.nc = nc
        self.prog = {e: [] for e in ENGS}
        self.acc = {}
        self.dma_n = {e: 0 for e in ENGS}
        self.dma_cnt = {}
        self.frozen = set()

    def freeze(self, tensor_ap):
        self.frozen.add(tensor_ap.tensor.name)

    def _track(self, ins, tok, reads, writes):
        deps = set()
        rb = [_box(a) for a in reads]
        wb = [_box(a) for a in writes]
        for bx in rb:
            lst = self.acc.setdefault(bx[0], [])
            psum = bx[0].startswith("pb")
            for (b2, w2, t2) in lst:
                if _overlap(bx, b2):
                    if w2:
                        deps.add(t2)
                    elif psum and t2[1] != ins.eng:
                        deps.add(t2)
        for bx in wb:
            lst = self.acc.setdefault(bx[0], [])
            for (b2, w2, t2) in lst:
                if _overlap(bx, b2):
                    deps.add(t2)
        for bx in rb:
            if bx[0] in self.frozen:
                continue
            lst = self.acc[bx[0]]
            if tok[0] == 'c':
                lst[:] = [r for r in lst if not ((not r[1]) and r[2][0] == 'c' and r[2][1] == tok[1]
                                                 and _covers(bx, r[0]))]
            lst.append((bx, False, tok))
        for bx in wb:
            lst = self.acc[bx[0]]
            lst[:] = [r for r in lst if not _covers(bx, r[0])]
            lst.append((bx, True, tok))
        deps.discard(tok)
        e = ins.eng
        if e == "pe" or not SAME_ENGINE_SYNC:
            deps = {d for d in deps if not (d[0] == 'c' and d[1] == e)}
        ins.deps = sorted(deps, key=str)

    def op(self, eng, fn, reads=(), writes=()):
        ins = _Ins(eng, fn)
        ins.idx = len(self.prog[eng])
        self.prog[eng].append(ins)
        self._track(ins, ('c', eng, ins.idx), reads, writes)
        return ins

    def dma(self, eng, out, in_, **kw):
        ins = _Ins(eng, lambda e: e.dma_start(out=out, in_=in_, **kw))
        ins.idx = len(self.prog[eng])
        self.prog[eng].append(ins)
        slot = self.dma_n[eng] % DMA_POOL
        self.dma_n[eng] += 1
        c = self.dma_cnt.get((eng, slot), 0) + 1
        self.dma_cnt[(eng, slot)] = c
        ins.dma = (slot, c)
        self._track(ins, ('d', eng, slot, c), [in_], [out])
        return ins

    def emit(self):
        nc = self.nc
        for e in ENGS:
            for ins in self.prog[e]:
                for d in ins.deps:
                    if d[0] == 'c':
                        self.prog[d[1]][d[2]].signal = True
        sigval = {}
        for e in ENGS:
            n = 0
            for ins in self.prog[e]:
                if ins.signal:
                    n += 1
                    sigval[(e, ins.idx)] = n
        with contextlib.ExitStack() as st:
            csem = {e: st.enter_context(nc.semaphore("c_" + e)) for e in ENGS}
            dsem = {}
            for e in ENGS:
                for s in range(min(DMA_POOL, self.dma_n[e])):
                    dsem[(e, s)] = st.enter_context(nc.semaphore("d_%s_%d" % (e, s)))
            block = st.enter_context(nc.Block())
            getter = {"pe": "tensor", "act": "scalar", "dve": "vector", "pool": "gpsimd", "sp": "sync"}

            def build(ename):
                def body(eobj):
                    waited_c = {}
                    waited_d = {}
                    for ins in self.prog[ename]:
                        for d in ins.deps:
                            if d[0] == 'c':
                                v = sigval[(d[1], d[2])]
                                if waited_c.get(d[1], 0) < v:
                                    eobj.wait_ge(csem[d[1]], v)
                                    waited_c[d[1]] = v
                            else:
                                key = (d[1], d[2])
                                v = d[3] * 16
                                if waited_d.get(key, 0) < v:
                                    eobj.wait_ge(dsem[key], v)
                                    waited_d[key] = v
                        if ins.dma is not None:
                            slot, c = ins.dma
                            key = (ename, slot)
                            if c > 1 and waited_d.get(key, 0) < (c - 1) * 16:
                                eobj.wait_ge(dsem[key], (c - 1) * 16)
                                waited_d[key] = (c - 1) * 16
                            r = ins.fn(eobj)
                            r.then_inc(dsem[key], 16)
                        else:
                            r = ins.fn(eobj)
                            if ins.signal:
                                r.then_inc(csem[ename], 1)
                    for (e2, s), c in self.dma_cnt.items():
                        if e2 == ename and waited_d.get((e2, s), 0) < c * 16:
                            eobj.wait_ge(dsem[(e2, s)], c * 16)
                return body

            for e in ENGS:
                if self.prog[e]:
                    getattr(block, getter[e])(build(e))


def host_consts(SL):
    NT = SL // 128
    bf = ml_dtypes.bfloat16
    c = {}
    c["c_identf"] = np.eye(128, dtype=np.float32)
    c["c_identb"] = np.eye(128).astype(bf)
    c["c_onesb"] = np.ones((128, 128)).astype(bf)
    c["c_onesf"] = np.ones((128, 128), np.float32)
    j = np.arange(128)[:, None]
    i = np.arange(128)[None, :]
    c["c_tri"] = (j <= i).astype(np.float32)
    c["c_masku"] = np.where(i >= j, 0.0, -1e5).astype(np.float32)
    c["c_maskls"] = np.where(j > i, 0.0, -1e5).astype(np.float32)
    c["c_maskus01"] = (i > j).astype(bf)
    c["c_causal"] = np.where(j <= i, 0.0, NEG).astype(bf)
    c["c_winold"] = np.where(j > i, 0.0, NEG).astype(bf)
    m = np.arange(128)[:, None, None]
    ti = np.arange(NT)[None, :, None]
    ql = np.arange(128)[None, None, :]
    t = ti * 128 + ql
    c["c_cmask"] = np.where((m >= 1) & (16 * m + 15 <= t), 0.0, NEG).astype(bf)
    b = np.arange(32)[:, None]
    key = np.arange(SL)[None, :]
    c["c_rneg"] = np.where((key // 64) == b, NEG, 0.0).astype(bf)
    tq = (np.arange(NT)[None, :, None] * 128 + np.arange(128)[:, None, None])
    cur = tq // 64
    jb = np.arange(32)[None, None, :]
    forced = (jb == 0) | (jb == cur) | (jb == cur - 1)
    valid = jb <= cur
    c["c_selbias"] = np.where(forced, 1e30, np.where(valid, 0.0, -1e30)).astype(np.float32)
    n = np.arange(128)[:, None] - 1
    cs = n * 16
    ss = np.arange(32)[None, :] * 64
    ov = ((cs < ss + 64) & (ss < cs + 32) & (n >= 0)).astype(np.float32)
    c["c_overlap"] = ov.astype(bf)
    idx = np.arange(128)
    cm = np.zeros((128, 14, 128), np.float32)
    for lev in range(7):
        b_ = 1 << lev
        same = (idx[:, None] // (2 * b_)) == (idx[None, :] // (2 * b_))
        mk = same & ((idx[:, None] % (2 * b_)) >= b_) & ((idx[None, :] % (2 * b_)) < b_)
        cm[:, 2 * lev, :] = mk
        cm[:, 2 * lev + 1, :] = mk.T
    c["c_merge"] = cm.astype(bf)
    return c


W_NAMES = ["norm_mix", "w_in", "cmp_pos_k", "cmp_pos_v", "cmp_k_w1", "cmp_k_b1", "cmp_k_w2", "cmp_v_w1",
           "cmp_v_b1", "cmp_v_w2", "nsa_norm", "gdn_conv", "gdn_a_log", "gdn_dt_bias", "gdn_norm", "w_out",
           "norm_ffn", "ffn_up", "ffn_conv", "ffn_down", "norm_final"]
W_SHAPES = {
    "norm_mix": (2, 1024), "w_in": (2, 1024, 3360), "cmp_pos_k": (2, 32, 64), "cmp_pos_v": (2, 32, 64),
    "cmp_k_w1": (2, 2048, 256), "cmp_k_b1": (2, 256), "cmp_k_w2": (2, 256, 64), "cmp_v_w1": (2, 2048, 256),
    "cmp_v_b1": (2, 256), "cmp_v_w2": (2, 256, 64), "nsa_norm": (2, 512), "gdn_conv": (2, 4, 1536),
    "gdn_a_log": (2, 4), "gdn_dt_bias": (2, 4), "gdn_norm": (2, 128), "w_out": (2, 1024, 1024),
    "norm_ffn": (2, 1024), "ffn_up": (2, 1024, 5632), "ffn_conv": (2, 3, 5632), "ffn_down": (2, 2816, 1024),
    "norm_final": (1024,),
}

C_Q = 0
C_KV = 512
C_GATE = 1280
C_GQKV = 1304
C_Z = 2840
C_B = 3352
C_A = 3356
NFM = 24
NTM = 288
RING_SLOT = 3072
NRING = 4


class _Stop(Exception):
    pass


STOP = None


def _chk(name):
    if STOP == name:
        raise _Stop()


class _Emitter:
    def __init__(self, S):
        self.S = S

    def __enter__(self):
        return self

    def __exit__(self, et, ev, tb):
        if et is not None and issubclass(et, _Stop):
            self.S.emit()
            return True
        return False


def build(nc, SL, NSEQ, DEPTH, dbg_names=()):
    NT = SL // 128
    NST = SL // 512
    L = DEPTH
    consts = host_consts(SL)

    x_d = nc.dram_tensor("x", [NSEQ, SL, D_MODEL], F32, kind="ExternalInput").ap()
    out_d = nc.dram_tensor("out", [NSEQ, SL, D_MODEL], F32, kind="ExternalOutput").ap()
    wd = {}
    for n in W_NAMES:
        wd[n] = nc.dram_tensor(n, list(W_SHAPES[n]), F32, kind="ExternalInput").ap()
    cd = {}
    for n, a in consts.items():
        cd[n] = nc.dram_tensor(n, list(a.shape), F32 if a.dtype == np.float32 else BF16, kind="ExternalInput").ap()
    s_wfm = nc.dram_tensor("s_wfm", [L, NFM, 128, 1024], BF16, kind="Internal").ap()
    s_wtm = nc.dram_tensor("s_wtm", [L, 128, 8 * NTM], BF16, kind="Internal").ap()
    s_wo = nc.dram_tensor("s_wo", [L, 8, 128, 1024], BF16, kind="Internal").ap()
    s_wup = nc.dram_tensor("s_wup", [L, 44, 128, 1024], BF16, kind="Internal").ap()
    s_wdn = nc.dram_tensor("s_wdn", [L, 8, 128, 22 * 128], BF16, kind="Internal").ap()
    s_w1 = nc.dram_tensor("s_w1", [L, 2, 4, 128, 8 * 256], BF16, kind="Internal").ap()
    dbg = {}
    for (n, shp) in dbg_names:
        dbg[n] = nc.dram_tensor(n, list(shp), F32, kind="ExternalOutput").ap()

    with contextlib.ExitStack() as st:
        def T(n, s, d):
            return st.enter_context(nc.sbuf_tensor(n, s, d))

        S = Sched(nc)

        def mm(out, lhsT, rhs, start=True, stop=True, skip=False):
            S.op("pe", lambda e: e.matmul(out, lhsT=lhsT, rhs=rhs, start=start, stop=stop, skip_group_check=skip),
                 [lhsT, rhs], [out])

        def trp(out, in_, ident):
            S.op("pe", lambda e: e.transpose(out=out, in_=in_, identity=ident), [in_, ident], [out])

        def act(out, in_, func, bias=None, scale=1.0, accum=None, eng="act"):
            rd = [in_]
            kw = {}
            if bias is not None:
                kw["bias"] = bias
                rd.append(bias)
            if not isinstance(scale, float):
                rd.append(scale)
            wr = [out]
            if accum is not None:
                kw["accum_out"] = accum
                wr.append(accum)
            S.op(eng, lambda e: e.activation(out=out, in_=in_, func=func, scale=scale, **kw), rd, wr)

        def tt(eng, out, in0, in1, op):
            S.op(eng, lambda e: e.tensor_tensor(out=out, in0=in0, in1=in1, op=op), [in0, in1], [out])

        def ts(eng, out, in0, s1, op0, s2=None, op1=None):
            rd = [in0]
            if not isinstance(s1, (float, int)):
                rd.append(s1)
            if s2 is not None and not isinstance(s2, (float, int)):
                rd.append(s2)
            if op1 is None:
                S.op(eng, lambda e: e.tensor_scalar(out=out, in0=in0, scalar1=s1, scalar2=None, op0=op0), rd, [out])
            else:
                S.op(eng, lambda e: e.tensor_scalar(out=out, in0=in0, scalar1=s1, scalar2=s2, op0=op0, op1=op1),
                     rd, [out])

        def stt(eng, out, in0, scalar, in1, op0, op1):
            rd = [in0, in1]
            if not isinstance(scalar, (float, int)):
                rd.append(scalar)
            S.op(eng, lambda e: e.scalar_tensor_tensor(out=out, in0=in0, scalar=scalar, in1=in1, op0=op0, op1=op1),
                 rd, [out])

        def cp(eng, out, in_):
            if eng == "act":
                act(out, in_, AF.Copy)
            else:
                S.op(eng, lambda e: e.tensor_copy(out=out, in_=in_), [in_], [out])

        def memset(eng, ap, v):
            S.op(eng, lambda e: e.memset(ap, v), [], [ap])

        def bc(ap, shape):
            return ap.broadcast_to(shape)

        ARENA = 32768
        arena = T("arena", [128, ARENA], BF16)

        def av(off_bytes, nbytes, dt):
            v = arena[:, off_bytes // 2:(off_bytes + nbytes) // 2]
            return v.bitcast(F32) if dt == F32 else v

        TMP = 32768
        qT = av(0, 8192, BF16).rearrange("p (g c t) -> p g c t", g=2, c=4)
        qkvc = av(8192, 12288, BF16).rearrange("p (c t) -> p c t", c=12)
        zs = av(20480, 4096, BF16).rearrange("p (c t) -> p c t", c=4)
        mixT = av(24576, 8192, BF16).rearrange("p (c t) -> p c t", c=8)
        sq1 = av(TMP, 8192, BF16).rearrange("p (c t) -> p c t", c=8)
        rstd1 = av(TMP + 8192, 2048, F32)
        xst_in = av(TMP + 10240, 4096, F32)
        rawc = [av(TMP + 14336 + k * 2064, 2060, F32) for k in range(2)]
        actT = av(0, 22528, BF16).rearrange("p (c t) -> p c t", c=22)
        fraw = [[av(22528 + (2 * k + h) * 2056, 2056, F32) for h in range(2)] for k in range(2)]
        fy = [[av(30752 + (2 * k + h) * 2048, 2048, F32) for h in range(2)] for k in range(2)]
        sq2 = av(38944, 8192, BF16).rearrange("p (c t) -> p c t", c=8)
        rstd2 = av(47136, 2048, F32)
        xst_out = av(49184, 4096, F32)
        fsil = [av(53280 + k * 1024, 1024, BF16) for k in range(2)]

        xT = T("xT", [128, 8, 512], F32)
        hT = T("hT", [128, 8, 512], BF16)
        kTc = [T("kTc%d" % l, [128, 2, SL], BF16) for l in range(L)]
        Vtok = [T("Vtok%d" % l, [128, NT, 2, 2, 65], BF16) for l in range(L)]
        kcmpb = [[T("kcmpb%d_%d" % (l, k), [128, 2, 16, 34], BF16) for k in range(2)] for l in range(L)]
        kcT = [T("kcT%d" % l, [128, 128], BF16) for l in range(L)]
        vcaug = [T("vcaug%d" % l, [128, 2, 128], BF16) for l in range(L)]
        halo_g = [T("halog%d" % l, [128, 12, 3], F32) for l in range(L)]
        halo_f = [T("halof%d" % l, [128, 44, 2], F32) for l in range(L)]
        Sst = [T("Sst%d" % l, [128, 4, 128], F32) for l in range(L)]
        Sbf = [T("Sbf%d" % l, [128, 4, 128], BF16) for l in range(L)]
        ring = T("ring", [128, NRING, RING_SLOT], BF16)
        gate_sb = T("gate_sb", [128, 4, 24], F32)
        beta_sb = T("beta_sb", [128, 4, 4], F32)
        g_sb = T("g_sb", [128, 4, 4], F32)
        identf = T("identf", [128, 128], F32)
        identb = T("identb", [128, 128], BF16)
        onesb = T("onesb", [128, 128], BF16)
        onesf = T("onesf", [128, 128], F32)
        tri = T("tri", [128, 128], F32)
        masku = T("masku", [128, 128], F32)
        maskls = T("maskls", [128, 128], F32)
        maskus01 = T("maskus01", [128, 128], BF16)
        causal = T("causal", [128, 128], BF16)
        winold = T("winold", [128, 128], BF16)
        cmask = T("cmask", [128, NT, 128], BF16)
        rneg = T("rneg", [32, SL], BF16)
        selbias = T("selbias", [128, NT, 32], F32)
        cmerge = T("cmerge", [128, 14, 128], BF16)
        epsb = T("epsb", [128, 1], F32)
        oneb = T("oneb", [128, 1], F32)
        inv1024 = T("inv1024", [128, 128], BF16)
        inv128 = T("inv128", [128, 128], BF16)
        cwg = [T("cwg%d" % l, [128, 12, 4], F32) for l in range(L)]
        cwf = [T("cwf%d" % l, [128, 44, 3], F32) for l in range(L)]
        cbias = [[T("cbias%d_%d" % (l, k), [128, 2], F32) for k in range(2)] for l in range(L)]
        w2k = [T("w2k%d" % l, [128, 2, 128], BF16) for l in range(L)]
        w2v = [T("w2v%d" % l, [128, 2, 64], BF16) for l in range(L)]
        dtb = [T("dtb%d" % l, [128, 4], F32) for l in range(L)]
        nexpa = [T("nexpa%d" % l, [128, 4], F32) for l in range(L)]
        gfin = T("gfin", [128, 8], F32)
        smallf = T("smallf", [128, 1024], F32)
        gtile = [T("gt%d" % k, [128, 8], F32) for k in range(4)]
        posbs = [[T("posb%d_%d" % (l, kv), [64, 32], BF16) for kv in range(2)] for l in range(L)]

        psb = [st.enter_context(nc.psum_tensor("pb%d" % i, [128, 512], F32)) for i in range(8)]
        rot = {"S": [0, 1], "O": [2, 3], "P": [4, 5], "M": [6, 7]}
        rot_i = {k: 0 for k in rot}

        def bank(role):
            b = rot[role][rot_i[role] % len(rot[role])]
            rot_i[role] += 1
            return psb[b]

        st.enter_context(_Emitter(S))
        cload = [("c_identf", identf), ("c_identb", identb), ("c_onesb", onesb), ("c_onesf", onesf), ("c_tri", tri),
                 ("c_masku", masku), ("c_maskls", maskls), ("c_maskus01", maskus01), ("c_causal", causal),
                 ("c_winold", winold), ("c_cmask", cmask), ("c_rneg", rneg), ("c_selbias", selbias),
                 ("c_merge", cmerge)]
        import os as _os
        _skip = (_os.environ.get("SKIP") or "").split(",")
        for n, t_ in cload:
            if n in _skip:
                continue
            S.dma("sp", t_[:], cd[n])
        if "memset" not in _skip:
            memset("pool", epsb[:], EPS)
            memset("pool", oneb[:], 1.0)
            memset("pool", inv1024[:], 1.0 / 1024.0)
            memset("pool", inv128[:], 1.0 / 128.0)
        if "gfin" not in _skip:
            S.dma("sp", gfin[:], wd["norm_final"].rearrange("(kc p) -> p kc", p=128), allow_slow_non_contiguous=True)

        _chk("const")
        stg_f = [av(k * 11264, 11264, F32) for k in range(2)]
        stg_b = [av(22528 + k * 5632, 5632, BF16) for k in range(2)]
        pp_n = [0]

        def prepass(src, dst, nk, ncol, gain=None):
            i = pp_n[0]
            pp_n[0] += 1
            sf = stg_f[i % 2][:, 0:nk * ncol].rearrange("p (k n) -> p k n", k=nk)
            sb = stg_b[i % 2][:, 0:nk * ncol].rearrange("p (k n) -> p k n", k=nk)
            S.dma("sp", sf, src)
            eng = "dve" if i % 2 == 0 else "pool"
            if gain is not None:
                tt(eng, sb, sf, bc(gain.unsqueeze(2), [128, nk, ncol]), ALU.mult)
            else:
                cp(eng, sb, sf)
            S.dma("act", dst, stg_b[i % 2][:, 0:nk * ncol])
            return stg_b[i % 2]

        for l in range(L):
            gmix, gffn, gout = gtile[0], gtile[1], gtile[2]
            S.dma("sp", gmix[:], wd["norm_mix"][l].rearrange("(kc p) -> p kc", p=128), allow_slow_non_contiguous=True)
            S.dma("sp", gffn[:], wd["norm_ffn"][l].rearrange("(kc p) -> p kc", p=128), allow_slow_non_contiguous=True)
            S.dma("sp", gout[:, 0:4], wd["nsa_norm"][l].rearrange("(kc p) -> p kc", p=128),
                  allow_slow_non_contiguous=True)
            for k in range(4):
                S.dma("sp", gout[:, 4 + k:5 + k], wd["gdn_norm"][l].rearrange("(p o) -> p o", o=1))
            win = wd["w_in"][l].rearrange("(kc p) n -> p kc n", p=128)
            for p in range(NFM):
                if p < 4:
                    i = pp_n[0]
                    pp_n[0] += 1
                    sf = stg_f[i % 2][:, 0:1024].rearrange("p (k n) -> p k n", k=8)
                    sb = stg_b[i % 2][:, 0:1024].rearrange("p (k n) -> p k n", k=8)
                    S.dma("sp", sf[:, :, 0:64], win[:, :, C_Q + 64 * p:C_Q + 64 * p + 64])
                    S.dma("sp", sf[:, :, 64:128], win[:, :, C_Q + 64 * (p + 4):C_Q + 64 * (p + 4) + 64])
                    eng = "dve" if i % 2 == 0 else "pool"
                    tt(eng, sb, sf, bc(gmix[:].unsqueeze(2), [128, 8, 128]), ALU.mult)
                    S.dma("act", s_wfm[l, p], stg_b[i % 2][:, 0:1024])
                    continue
                if p == 4:
                    c0 = C_KV + 0 * 128
                elif p == 5:
                    c0 = C_KV + 2 * 128
                elif p == 6:
                    c0 = C_KV + 4 * 128
                elif p == 7:
                    c0 = C_KV + 1 * 128
                elif p < 20:
                    c0 = C_GQKV + 128 * (p - 8)
                else:
                    c0 = C_Z + 128 * (p - 20)
                prepass(win[:, :, c0:c0 + 128], s_wfm[l, p], 8, 128, gmix[:])
            i = pp_n[0]
            pp_n[0] += 1
            sf = stg_f[i % 2][:, 0:8 * NTM].rearrange("p (k n) -> p k n", k=8)
            sb = stg_b[i % 2][:, 0:8 * NTM].rearrange("p (k n) -> p k n", k=8)
            S.dma("sp", sf[:, :, 0:128], win[:, :, C_KV + 3 * 128:C_KV + 4 * 128])
            S.dma("sp", sf[:, :, 128:256], win[:, :, C_KV + 5 * 128:C_KV + 6 * 128])
            S.dma("sp", sf[:, :, 256:280], win[:, :, C_GATE:C_GATE + 24])
            S.dma("sp", sf[:, :, 280:288], win[:, :, C_B:C_B + 8])
            tt("dve", sb, sf, bc(gmix[:].unsqueeze(2), [128, 8, NTM]), ALU.mult)
            S.dma("act", s_wtm[l], stg_b[i % 2][:, 0:8 * NTM])
            wo = wd["w_out"][l].rearrange("(kc p) n -> p kc n", p=128)
            for d in range(8):
                prepass(wo[:, :, 128 * d:128 * d + 128], s_wo[l, d], 8, 128, gout[:])
            wu = wd["ffn_up"][l].rearrange("(kc p) n -> p kc n", p=128)
            for c in range(22):
                prepass(wu[:, :, 128 * c:128 * c + 128], s_wup[l, 2 * c], 8, 128, gffn[:])
                prepass(wu[:, :, D_FF + 128 * c:D_FF + 128 * c + 128], s_wup[l, 2 * c + 1], 8, 128, gffn[:])
            wdn = wd["ffn_down"][l].rearrange("(kc p) n -> p kc n", p=128)
            for d in range(8):
                prepass(wdn[:, :, 128 * d:128 * d + 128], s_wdn[l, d], 22, 128, None)
            for kv, (w1n, b1n, posn) in enumerate((("cmp_k_w1", "cmp_k_b1", "cmp_pos_k"),
                                                   ("cmp_v_w1", "cmp_v_b1", "cmp_pos_v"))):
                w1 = wd[w1n][l].rearrange("(l d) h -> d l h", d=64)
                posf = smallf[0:64, 0:32]
                S.dma("sp", posf, wd[posn][l].rearrange("l d -> d l"), allow_slow_non_contiguous=True)
                posb = posbs[l][kv]
                cp("dve", posb[:], posf)
                pcb = bank("M")
                for pc in range(4):
                    i = pp_n[0]
                    pp_n[0] += 1
                    sf = stg_f[i % 2][:, 0:2048].rearrange("p (k n) -> p k n", k=8)
                    sb = stg_b[i % 2][:, 0:2048].rearrange("p (k n) -> p k n", k=8)
                    S.dma("sp", sf[0:64], w1[:, 8 * pc:8 * pc + 8, :])
                    S.dma("sp", sf[64:128], w1[:, 8 * pc:8 * pc + 8, :])
                    cp("dve" if i % 2 == 0 else "pool", sb, sf)
                    S.dma("act", s_w1[l, kv, pc], stg_b[i % 2][:, 0:2048])
                    for ll in range(8):
                        for hc in range(2):
                            mm(pcb[:, hc:hc + 1], sb[0:64, ll, 128 * hc:128 * hc + 128],
                               posb[:, 8 * pc + ll:8 * pc + ll + 1],
                               start=(pc == 0 and ll == 0 and hc == 0), stop=(pc == 3 and ll == 7), skip=True)
                b1t = smallf[:, 64:66]
                S.dma("sp", b1t, wd[b1n][l].rearrange("(hc p) -> p hc", p=128), allow_slow_non_contiguous=True)
                tt("dve", cbias[l][kv][:], pcb[:, 0:2], b1t, ALU.add)
            w2f = smallf[:, 128:256].rearrange("p (hc n) -> p hc n", hc=2)
            S.dma("sp", w2f, wd["cmp_k_w2"][l].rearrange("(hc p) n -> p hc n", p=128))
            cp("dve", w2k[l][:, :, 0:64], w2f)
            cp("dve", w2k[l][:, :, 64:128], w2f)
            w2f2 = smallf[:, 256:384].rearrange("p (hc n) -> p hc n", hc=2)
            S.dma("sp", w2f2, wd["cmp_v_w2"][l].rearrange("(hc p) n -> p hc n", p=128))
            cp("dve", w2v[l][:], w2f2)
            cg = av(34816 + 22528, 6144, F32)
            S.dma("sp", cg[0:4, :], wd["gdn_conv"][l])
            pcw = bank("M")
            for jj in range(12):
                mm(pcw[:, 4 * jj:4 * jj + 4], cg[0:4, 128 * jj:128 * jj + 128], identf[0:4, 0:4])
            cp("dve", cwg[l][:].rearrange("p j k -> p (j k)"), pcw[:, 0:48])
            cf = av(34816, 22528, F32)
            S.dma("sp", cf[0:3, :], wd["ffn_conv"][l])
            pcw2 = bank("M")
            for jj in range(44):
                c = jj // 2
                col = (128 * c) if jj % 2 == 0 else (D_FF + 128 * c)
                mm(pcw2[:, 3 * jj:3 * jj + 3], cf[0:3, col:col + 128], identf[0:3, 0:3])
            cp("dve", cwf[l][:].rearrange("p j k -> p (j k)"), pcw2[:, 0:132])
            S.dma("sp", dtb[l][:], wd["gdn_dt_bias"][l].partition_broadcast(128))
            S.dma("sp", nexpa[l][:], wd["gdn_a_log"][l].partition_broadcast(128))
            act(nexpa[l][:], nexpa[l][:], AF.Exp)
            act(dtb[l][:], dtb[l][:], AF.Exp)
            ts("dve", nexpa[l][:], nexpa[l][:], -1.0, ALU.mult)

        _chk("prepass")
        ring_i = [0]

        def wload(src, nelem):
            s = ring_i[0] % NRING
            ring_i[0] += 1
            dst = ring[:, s, 0:nelem]
            if len(src.shape) == 3:
                S.dma("sp", dst.rearrange("p (a n) -> p a n", a=src.shape[1]), src)
            else:
                S.dma("sp", dst, src)
            return dst

        def rmsnorm_to_hT(sqb, rstdb, inv, extra_gain=None):
            act(sqb, xT[:], AF.Square)
            pm = bank("P")
            for kc in range(8):
                mm(pm[:], inv[:], sqb[:, kc, :], start=(kc == 0), stop=(kc == 7))
            act(rstdb, pm[:], AF.Ln, bias=epsb[:])
            act(rstdb, rstdb, AF.Exp, scale=-0.5)

        for sq_i in range(NSEQ):
            for l in range(L):
                memset("pool", kcT[l][:], 0.0)
                memset("pool", vcaug[l][:], 0.0)
                memset("pool", kcmpb[l][0][:], 0.0)
                memset("pool", kcmpb[l][1][:], 0.0)
                memset("pool", halo_g[l][:], 0.0)
                memset("pool", halo_f[l][:], 0.0)
                memset("pool", Sst[l][:], 0.0)
                memset("pool", Sbf[l][:], 0.0)
                memset("pool", Vtok[l][:], 1.0)
                S.dma("sp", vcaug[l][:, 0, 96:128], cd["c_overlap"])
                S.dma("sp", vcaug[l][:, 1, 96:128], cd["c_overlap"])
                memset("pool", vcaug[l][:, :, 64:65], 1.0)
            for s_i in range(NST):
                t0 = 512 * s_i
                for tl in range(4):
                    S.dma("sp", xst_in, x_d[sq_i, t0 + 128 * tl:t0 + 128 * tl + 128, :])
                    for half in range(2):
                        pt = bank("M")
                        for k4 in range(4):
                            kc = 4 * half + k4
                            trp(pt[:, 128 * k4:128 * k4 + 128], xst_in[:, 128 * kc:128 * kc + 128], identf[:])
                        cp("act" if half == 0 else "dve", xT[:, 4 * half:4 * half + 4, 128 * tl:128 * tl + 128],
                           pt[:].rearrange("p (k t) -> p k t", k=4))
                _chk("P0")
                for l in range(L):
                    rmsnorm_to_hT(sq1, rstd1, inv1024)
                    _chk("P1a")
                    tt("dve", hT[:], xT[:], bc(rstd1.unsqueeze(1), [128, 8, 512]), ALU.mult)
                    _chk("P1b")
                    ri = 0
                    for p0 in range(0, NFM, 3):
                        slot = wload(s_wfm[l, p0:p0 + 3].rearrange("a p n -> p a n"), 3072)
                        slot = slot.rearrange("p (a k n) -> p a k n", a=3, k=8)
                        for a in range(3):
                            p = p0 + a
                            pp = bank("P")
                            for kc in range(8):
                                mm(pp[:], slot[:, a, kc, :], hT[:, kc, :], start=(kc == 0), stop=(kc == 7))
                            if p < 4:
                                if p == 0:
                                    memset("pool", qT[64:128, 0], 0.0)
                                    memset("pool", qT[0:64, 1], 0.0)
                                cp("act", qT[0:64, 0, p, :], pp[0:64, :])
                                cp("act", qT[64:128, 1, p, :], pp[64:128, :])
                            elif p == 4 or p == 7:
                                for g_ in range(2):
                                    cp("act", kcmpb[l][0 if p == 4 else 1][64 * g_:64 * g_ + 64, g_, :, 1:33],
                                       pp[64 * g_:64 * g_ + 64, :].rearrange("p (m r) -> p r m", r=16))
                            elif p == 5 or p == 6:
                                cp("act", kTc[l][:, p - 5, t0:t0 + 512], pp[:])
                            elif p < 20:
                                j = p - 8
                                rw = rawc[ri % 2]
                                ri += 1
                                cp("pool", rw[:, 0:3], halo_g[l][:, j, :])
                                cp("act", rw[:, 3:515], pp[:])
                                cp("pool", halo_g[l][:, j, :], rw[:, 512:515])
                                y = fy[0][ri % 2]
                                act(y, pp[:], AF.Copy, scale=cwg[l][:, j, 3:4])
                                for k in range(0, 3):
                                    stt("dve", y, rw[:, k:k + 512], cwg[l][:, j, k:k + 1], y, ALU.mult, ALU.add)
                                act(qkvc[:, j, :], y, AF.Silu)
                            else:
                                act(zs[:, p - 20, :], pp[:], AF.Silu)
                        if p0 == 0:
                            _chk("P1c")
                        if p0 == 9:
                            _chk("P1c2")
                    _chk("P1d")
                    wt = wload(s_wtm[l], 8 * NTM).rearrange("p (k n) -> p k n", k=8)
                    for tl in range(4):
                        pp = bank("P")
                        for kc in range(8):
                            mm(pp[:, 0:NTM], hT[:, kc, 128 * tl:128 * tl + 128], wt[:, kc, :],
                               start=(kc == 0), stop=(kc == 7))
                        gt = 4 * s_i + tl
                        cp("act", Vtok[l][:, gt, :, :, 0:64],
                           pp[:, 0:256].rearrange("p (b h d) -> p b h d", b=2, h=2))
                        _chk("P1e1")
                        act(gate_sb[:, tl, :], pp[:, 256:280], AF.Sigmoid)
                        act(beta_sb[:, tl, :], pp[:, 280:284], AF.Sigmoid)
                        _chk("P1e2")
                        act(g_sb[:, tl, :], pp[:, 284:288], AF.Exp)
                        tt("dve", g_sb[:, tl, :], g_sb[:, tl, :], dtb[l][:], ALU.mult)
                        _chk("P1e4")
                        act(g_sb[:, tl, :], g_sb[:, tl, :], AF.Ln, bias=oneb[:])
                        _chk("P1e5")
                        tt("dve", g_sb[:, tl, :], g_sb[:, tl, :], nexpa[l][:], ALU.mult)

                    _chk("P1")
                    hid = av(TMP, 512, F32).rearrange("p (a b n) -> p a b n", a=2, b=2)
                    hin = av(TMP + 512, 512, F32).rearrange("p (a b n) -> p a b n", a=2, b=2)
                    gel = av(TMP + 1024, 256, BF16).rearrange("p (a b n) -> p a b n", a=2, b=2)
                    for kv in range(2):
                        for pc in range(4):
                            w1s = wload(s_w1[l, kv, pc], 2048).rearrange("p (k n) -> p k n", k=8)
                            _chk("P3w")
                            ph = bank("S")
                            phv = ph[:, 0:128].rearrange("p (a b n) -> p a b n", a=2, b=2)
                            for hc in range(2):
                                for hd in range(2):
                                    for ll in range(8):
                                        lt = 8 * pc + ll
                                        rhs = kcmpb[l][kv][:, hd, lt % 16, lt // 16:lt // 16 + 32]
                                        mm(phv[:, hc, hd, :], w1s[:, ll, 128 * hc:128 * hc + 128],
                                           rhs, start=(ll == 0), stop=(ll == 7))
                                    if hc == 0 and hd == 0:
                                        _chk("P3m0")
                                    if hc == 0 and hd == 1:
                                        _chk("P3m1")
                            if pc == 0:
                                cp("act", hid.rearrange("p a b n -> p (a b n)"), ph[:, 0:128])
                                for hc in range(2):
                                    ts("dve", hid[:, hc].rearrange("p b n -> p (b n)"),
                                       hid[:, hc].rearrange("p b n -> p (b n)"), cbias[l][kv][:, hc:hc + 1], ALU.add)
                            else:
                                tt("dve", hid.rearrange("p a b n -> p (a b n)"), hid.rearrange("p a b n -> p (a b n)"),
                                   ph[:, 0:128], ALU.add)
                        _chk("P3a")
                        hidf = hid.rearrange("p a b n -> p (a b n)")
                        hinf = hin.rearrange("p a b n -> p (a b n)")
                        tt("dve", hinf, hidf, hidf, ALU.mult)
                        ts("dve", hinf, hinf, 0.044715, ALU.mult, 1.0, ALU.add)
                        tt("dve", hinf, hinf, hidf, ALU.mult)
                        act(hinf, hinf, AF.Sigmoid, scale=1.5957691216)
                        tt("dve", gel.rearrange("p a b n -> p (a b n)"), hinf, hidf, ALU.mult)
                        _chk("P3b")
                        cp("pool", kcmpb[l][kv][:, :, :, 0], kcmpb[l][kv][:, :, :, 32])
                        _chk("P3c")
                        if kv == 0:
                            for hd in range(2):
                                pk = bank("M")
                                for hc in range(2):
                                    mm(pk[:, 0:32], w2k[l][:, hc, :], gel[:, hc, hd, :], start=(hc == 0), stop=(hc == 1))
                                cp("act", kcT[l][64 * hd:64 * hd + 64, 32 * s_i:32 * s_i + 32],
                                   pk[64 * hd:64 * hd + 64, 0:32])
                        else:
                            for hd in range(2):
                                pv = bank("M")
                                gpad = av(TMP + 2560 + hd * 512, 512, BF16).rearrange("p (a n) -> p a n", a=2)
                                memset("pool", gpad, 0.0)
                                cp("pool", gpad[:, :, 32 * s_i % 128:32 * s_i % 128 + 32], gel[:, :, hd, :])
                                for hc in range(2):
                                    mm(pv[:, 0:64], gpad[:, hc, :], w2v[l][:, hc, :], start=(hc == 0), stop=(hc == 1))
                                tt("dve", vcaug[l][:, hd, 0:64], vcaug[l][:, hd, 0:64], pv[:, 0:64], ALU.add)

                    _chk("P3")
                    for tl in range(4):
                        gt = 4 * s_i + tl
                        q0 = 128 * tl
                        Eb = [av(TMP + k * 1024, 1024, BF16) for k in range(3)]
                        O_sb = av(TMP + 3072, 6144, F32).rearrange("p (b h d) -> p b h d", b=3, h=8)
                        O_tmp = av(TMP + 9216, 6144, F32).rearrange("p (b h d) -> p b h d", b=3, h=8)
                        o_f = av(TMP + 15360, 2048, F32)
                        o_b = av(TMP + 17408, 1024, BF16)
                        den = av(TMP + 18432, 96, F32).rearrange("p (b h) -> p b h", b=3)
                        coef = av(TMP + 18528, 96, F32).rearrange("p (b h) -> p b h", b=3)
                        impn = av(TMP + 18624, 1024, F32).rearrange("p (g c j) -> p g c j", g=2, c=4)
                        score = av(TMP + 19648, 256, F32).rearrange("p (g j) -> p g j", g=2)
                        top8 = av(TMP + 19904, 64, F32).rearrange("p (g j) -> p g j", g=2)
                        nsel = av(TMP + 19968, 256, F32).rearrange("p (g j) -> p g j", g=2)
                        nselT = [av(TMP + 20224 + g * 256, 256, BF16) for g in range(2)]
                        ssq = av(TMP + 20736, 4, F32)
                        junk = av(TMP + 20800, 2048, F32)
                        ei = [0]

                        def branch(g, br, chunks):
                            ncol = 128 if br == 0 else 65
                            po = bank("O")
                            pov = po[:].rearrange("p (c n) -> p c n", c=4)[:, :, 0:ncol]
                            qrhs = qT[:, g, :, q0:q0 + 128]
                            n = len(chunks)
                            for ci, (kl, masks, vr) in enumerate(chunks):
                                ps = bank("S")
                                psv = ps[:].rearrange("p (c t) -> p c t", c=4)
                                mm(psv, kl, qrhs, start=True, stop=(len(masks) == 0))
                                for mi, (ml, mr) in enumerate(masks):
                                    mm(psv, ml, mr, start=False, stop=(mi == len(masks) - 1))
                                E = Eb[ei[0] % 3]
                                ei[0] += 1
                                act(E, ps[:], AF.Exp, scale=0.125)
                                Ev = E.rearrange("p (c t) -> p c t", c=4)
                                for c in range(4):
                                    mm(pov[:, c, :], Ev[:, c, :], vr, start=(ci == 0 and c == 0), stop=(ci == n - 1),
                                       skip=True)
                            cp("act", O_sb[:, br, 4 * g:4 * g + 4, :], pov[:, :, 0:64])
                            cp("dve", den[:, br, 4 * g:4 * g + 4], pov[:, :, 64])
                            return pov

                        for g in range(2):
                            cmr = bc(cmask[:, gt, :].unsqueeze(1), [128, 4, 128])
                            pov = branch(g, 0, [(kcT[l][:, :], [(identb[:], cmr)], vcaug[l][:, g, :])])
                            dcl = av(TMP + 20740 + 16 * g, 16, F32)
                            ts("dve", dcl, den[:, 0, 4 * g:4 * g + 4], 1e-30, ALU.max)
                            S.op("dve", (lambda d_: (lambda e: e.reciprocal(out=d_, in_=d_)))(dcl), [dcl], [dcl])
                            tt("dve", impn[:, g], pov[:, :, 96:128], bc(dcl.unsqueeze(2), [128, 4, 32]), ALU.mult)
                            S.op("dve", (lambda o_, i_: (lambda e: e.tensor_reduce(out=o_, in_=i_, axis=AX.X, op=ALU.add)))(
                                score[:, g, :], impn[:, g].rearrange("p c j -> p j c")),
                                [impn[:, g]], [score[:, g, :]])
                        for g in range(2):
                            chunks = []
                            for kc in range(gt - 4, gt + 1):
                                if kc < 0:
                                    continue
                                masks = []
                                if kc == gt - 4:
                                    masks = [(identb[:], bc(winold[:].unsqueeze(1), [128, 4, 128]))]
                                elif kc == gt:
                                    masks = [(identb[:], bc(causal[:].unsqueeze(1), [128, 4, 128]))]
                                chunks.append((kTc[l][:, 1, 128 * kc:128 * kc + 128], masks,
                                               Vtok[l][:, kc, 1, g, :]))
                            branch(g, 2, chunks)
                        for g in range(2):
                            tt("dve", score[:, g, :], score[:, g, :], selbias[:, gt, :], ALU.add)
                            S.op("dve", (lambda o_, i_: (lambda e: e.max(out=o_, in_=i_)))(top8[:, g, :], score[:, g, :]),
                                 [score[:, g, :]], [top8[:, g, :]])
                            ts("dve", top8[:, g, 7:8], top8[:, g, 7:8], -1e29, ALU.max)
                            ts("dve", nsel[:, g, :], score[:, g, :], top8[:, g, 7:8], ALU.is_lt)
                            pt = bank("M")
                            trp(pt[0:32, 0:128], nsel[:, g, :], identf[:])
                            cp("dve", nselT[g][0:32, :], pt[0:32, 0:128])
                        for g in range(2):
                            chunks = []
                            for kc in range(0, gt + 1):
                                masks = [(rneg[0:32, 128 * kc:128 * kc + 128],
                                          bc(nselT[g][0:32, :].unsqueeze(1), [32, 4, 128]))]
                                if kc == gt:
                                    masks.append((identb[:], bc(causal[:].unsqueeze(1), [128, 4, 128])))
                                chunks.append((kTc[l][:, 0, 128 * kc:128 * kc + 128], masks,
                                               Vtok[l][:, kc, 0, g, :]))
                            branch(g, 1, chunks)
                        denf = den.rearrange("p b h -> p (b h)")
                        ts("dve", denf, denf, 1e-30, ALU.max)
                        S.op("dve", (lambda d_: (lambda e: e.reciprocal(out=d_, in_=d_)))(denf), [denf], [denf])
                        tt("dve", coef, den, gate_sb[:, tl, :].rearrange("p (h b) -> p b h", b=3), ALU.mult)
                        tt("dve", O_tmp, O_sb, bc(coef.unsqueeze(3), [128, 3, 8, 64]), ALU.mult)
                        ofv = o_f.rearrange("p (h d) -> p h d", h=8)
                        tt("dve", ofv, O_tmp[:, 0], O_tmp[:, 1], ALU.add)
                        tt("dve", ofv, ofv, O_tmp[:, 2], ALU.add)
                        act(junk, o_f, AF.Square, accum=ssq)
                        ts("dve", ssq, ssq, 1.0 / 512.0, ALU.mult)
                        act(ssq, ssq, AF.Ln, bias=epsb[:])
                        act(ssq, ssq, AF.Exp, scale=-0.5)
                        ts("dve", o_b, o_f, ssq, ALU.mult)
                        pt = bank("M")
                        ptb = pt[:].bitcast(BF16)
                        for c in range(4):
                            trp(ptb[:, 128 * c:128 * c + 128], o_b[:, 128 * c:128 * c + 128], identb[:])
                        cp("act", mixT[:, 0:4, q0:q0 + 128], ptb[:, 0:512].rearrange("p (c t) -> p c t", c=4))

                    _chk("P4")
                    G0 = TMP
                    sqg = av(G0, 8192, BF16).rearrange("p (c t) -> p c t", c=8)
                    rn = av(G0 + 8192, 2048, F32)
                    act(sqg, qkvc[:, 0:8, :], AF.Square)
                    for j in range(8):
                        pm = bank("P")
                        mm(pm[:], onesb[:], sqg[:, j, :])
                        act(rn, pm[:], AF.Ln, bias=epsb[:])
                        act(rn, rn, AF.Exp, scale=-0.5)
                        if j < 4:
                            stt("dve", qkvc[:, j, :], qkvc[:, j, :], float(128 ** -0.5), rn, ALU.mult, ALU.mult)
                        else:
                            tt("dve", qkvc[:, j, :], qkvc[:, j, :], rn, ALU.mult)

                    def gv(off, dt):
                        n = 2048 if dt == F32 else 1024
                        return av(G0 + off, n, dt).rearrange("p (h t) -> p h t", h=4)

                    F1 = gv(0, F32)
                    F2 = gv(2048, F32)
                    F3 = gv(4096, F32)
                    F4 = gv(6144, F32)
                    F5 = gv(8192, F32)
                    Bn = {}
                    for bi, nm in enumerate(["Bd", "kbT", "kbgT", "qgT", "attnT", "AT", "A", "TT", "Ao0", "AoT0", "Ao1",
                                             "AoT1", "kdec", "X", "vnew", "sqo", "Tn", "M1", "M1p"]):
                        Bn[nm] = gv(10240 + 1024 * bi, BF16)
                    gct = av(G0 + 29696, 16, F32)
                    edl = av(G0 + 29712, 16, F32)
                    for tl in range(4):
                        q0 = 128 * tl
                        gtk = g_sb[:, tl, :]
                        btk = beta_sb[:, tl, :]
                        pg = bank("M")
                        mm(pg[:, 0:4], tri[:], gtk)
                        cp("dve", gct, pg[:, 0:4])
                        tt("dve", F1, bc(tri[:].unsqueeze(1), [128, 4, 128]), bc(gtk.unsqueeze(2), [128, 4, 128]),
                           ALU.mult)
                        pgb = bank("S")
                        pgbv = pgb[:].rearrange("p (h t) -> p h t", h=4)
                        mm(pgb[:], onesf[:], F1.rearrange("p h t -> p (h t)"))
                        tt("dve", F2, pgbv, bc(gct.unsqueeze(2), [128, 4, 128]), ALU.subtract)
                        tt("dve", F2, F2, bc(masku[:].unsqueeze(1), [128, 4, 128]), ALU.add)
                        tt("dve", F3, bc(gct.unsqueeze(2), [128, 4, 128]), pgbv, ALU.subtract)
                        tt("dve", F3, F3, bc(maskls[:].unsqueeze(1), [128, 4, 128]), ALU.add)
                        act(F2, F2, AF.Exp)
                        act(F3, F3, AF.Exp)
                        act(F4, pgbv, AF.Exp)
                        act(F5, pgbv, AF.Copy)
                        tt("dve", edl, F5[:, :, 127], gct, ALU.subtract)
                        act(edl, edl, AF.Exp)
                        tt("dve", Bn["Bd"], bc(identb[:].unsqueeze(1), [128, 4, 128]),
                           bc(btk.unsqueeze(2), [128, 4, 128]), ALU.mult)
                        pbb = bank("P")
                        pbbv = pbb[:].rearrange("p (h t) -> p h t", h=4)
                        mm(pbb[:], onesb[:], Bn["Bd"].rearrange("p h t -> p (h t)"))
                        kn = qkvc[:, 4:8, q0:q0 + 128]
                        qn = qkvc[:, 0:4, q0:q0 + 128]
                        vv = qkvc[:, 8:12, q0:q0 + 128]
                        tt("dve", Bn["kbT"], kn, pbbv, ALU.mult)
                        tt("dve", F5, F4, pbbv, ALU.mult)
                        tt("dve", Bn["kbgT"], kn, F5, ALU.mult)
                        tt("dve", Bn["qgT"], qn, F4, ALU.mult)
                        p1 = bank("S")
                        p2 = bank("O")
                        p3 = bank("O")
                        p1v = p1[:].rearrange("p (h t) -> p h t", h=4)
                        p2v = p2[:].rearrange("p (h t) -> p h t", h=4)
                        p3v = p3[:].rearrange("p (h t) -> p h t", h=4)
                        for h in range(4):
                            mm(p1v[:, h, :], kn[:, h, :], Bn["kbT"][:, h, :])
                            mm(p2v[:, h, :], kn[:, h, :], qn[:, h, :])
                            mm(p3v[:, h, :], Bn["kbT"][:, h, :], kn[:, h, :])
                        tt("dve", Bn["attnT"], p2v, F2, ALU.mult)
                        tt("dve", Bn["AT"], p1v, F2, ALU.mult)
                        tt("dve", Bn["AT"], Bn["AT"], bc(maskus01[:].unsqueeze(1), [128, 4, 128]), ALU.mult)
                        tt("dve", Bn["A"], p3v, F3, ALU.mult)
                        def mk(i):
                            return bc(cmerge[:, i, :].unsqueeze(1), [128, 4, 128])
                        idb4 = bc(identb[:].unsqueeze(1), [128, 4, 128])
                        tt("pool", Bn["Ao0"], Bn["A"], mk(0), ALU.mult)
                        tt("pool", Bn["AoT0"], Bn["AT"], mk(1), ALU.mult)
                        tt("dve", Bn["Tn"], idb4, Bn["Ao0"], ALU.subtract)
                        tt("dve", Bn["TT"], idb4, Bn["AoT0"], ALU.subtract)
                        for lev in range(1, 7):
                            last = (lev == 6)
                            Ao = Bn["Ao%d" % (lev % 2)]
                            AoT = Bn["AoT%d" % (lev % 2)]
                            tt("pool", Ao, Bn["A"], mk(2 * lev), ALU.mult)
                            if not last:
                                tt("pool", AoT, Bn["AT"], mk(2 * lev + 1), ALU.mult)
                            pa = bank("S")
                            pav = pa[:].rearrange("p (h t) -> p h t", h=4)
                            for h in range(4):
                                mm(pav[:, h, :], Ao[:, h, :], Bn["TT"][:, h, :])
                            cp("act", Bn["M1"], pav)
                            if not last:
                                pb_ = bank("O")
                                pbv = pb_[:].rearrange("p (h t) -> p h t", h=4)
                                for h in range(4):
                                    mm(pbv[:, h, :], AoT[:, h, :], Bn["Tn"][:, h, :])
                                cp("dve", Bn["M1p"], pbv)
                            pc_ = bank("P")
                            pcv = pc_[:].rearrange("p (h t) -> p h t", h=4)
                            for h in range(4):
                                mm(pcv[:, h, :], Bn["Tn"][:, h, :], Bn["M1"][:, h, :])
                            if not last:
                                pd_ = bank("M")
                                pdv_ = pd_[:].rearrange("p (h t) -> p h t", h=4)
                                for h in range(4):
                                    mm(pdv_[:, h, :], Bn["TT"][:, h, :], Bn["M1p"][:, h, :])
                            tt("dve", Bn["TT"], Bn["TT"], pcv, ALU.subtract)
                            if not last:
                                tt("dve", Bn["Tn"], Bn["Tn"], pdv_, ALU.subtract)
                        pk = bank("M")
                        pkb = pk[:].bitcast(BF16)
                        for h in range(4):
                            trp(pkb[:, 128 * h:128 * h + 128], kn[:, h, :], identb[:])
                        tt("dve", Bn["kdec"], pkb[:, 0:512].rearrange("p (h t) -> p h t", h=4),
                           bc(edl.unsqueeze(2), [128, 4, 128]), ALU.mult)
                        pv_ = bank("M")
                        pvb = pv_[:].bitcast(BF16)
                        for h in range(4):
                            trp(pvb[:, 128 * h:128 * h + 128], vv[:, h, :], identb[:])
                        tt("dve", F1, pvb[:, 0:512].rearrange("p (h t) -> p h t", h=4),
                           bc(btk.unsqueeze(2), [128, 4, 128]), ALU.mult)
                        px = bank("S")
                        pxv = px[:].rearrange("p (h t) -> p h t", h=4)
                        for h in range(4):
                            mm(pxv[:, h, :], Bn["kbgT"][:, h, :], Sbf[l][:, h, :])
                        tt("dve", Bn["X"], F1, pxv, ALU.subtract)
                        pn = bank("O")
                        pnv = pn[:].rearrange("p (h t) -> p h t", h=4)
                        for h in range(4):
                            mm(pnv[:, h, :], Bn["TT"][:, h, :], Bn["X"][:, h, :])
                        cp("act", Bn["vnew"], pnv)
                        po_ = bank("P")
                        pov_ = po_[:].rearrange("p (h t) -> p h t", h=4)
                        for h in range(4):
                            mm(pov_[:, h, :], Sbf[l][:, h, :], Bn["qgT"][:, h, :], start=True, stop=False)
                            mm(pov_[:, h, :], Bn["vnew"][:, h, :], Bn["attnT"][:, h, :], start=False, stop=True)
                        pd = bank("S")
                        pdv = pd[:].rearrange("p (h t) -> p h t", h=4)
                        for h in range(4):
                            mm(pdv[:, h, :], Bn["kdec"][:, h, :], Bn["vnew"][:, h, :])
                        tt("dve", Sst[l][:], Sst[l][:], bc(F4[:, :, 127:128], [128, 4, 128]), ALU.mult)
                        tt("dve", Sst[l][:], Sst[l][:], pdv, ALU.add)
                        cp("act", Sbf[l][:], Sst[l][:])
                        act(Bn["sqo"], pov_, AF.Square)
                        pm = bank("M")
                        pmv = pm[:].rearrange("p (h t) -> p h t", h=4)
                        mm(pm[:], inv128[:], Bn["sqo"].rearrange("p h t -> p (h t)"))
                        act(F3, pmv, AF.Ln, bias=epsb[:])
                        act(F3, F3, AF.Exp, scale=-0.5)
                        tt("dve", F3, F3, pov_, ALU.mult)
                        tt("dve", mixT[:, 4:8, q0:q0 + 128], F3, zs[:, :, q0:q0 + 128], ALU.mult)

                    _chk("P5")
                    for d0 in range(0, 8, 3):
                        nd = min(3, 8 - d0)
                        slot = wload(s_wo[l, d0:d0 + nd].rearrange("a p n -> p a n"), nd * 1024)
                        slot = slot.rearrange("p (a k n) -> p a k n", a=nd, k=8)
                        for a in range(nd):
                            d = d0 + a
                            pp = bank("P")
                            for kc in range(8):
                                mm(pp[:], slot[:, a, kc, :], mixT[:, kc, :], start=(kc == 0), stop=(kc == 7))
                            tt("dve", xT[:, d, :], xT[:, d, :], pp[:], ALU.add)

                    _chk("P6")
                    rmsnorm_to_hT(sq2, rstd2, inv1024)
                    tt("dve", hT[:], xT[:], bc(rstd2.unsqueeze(1), [128, 8, 512]), ALU.mult)
                    for c in range(22):
                        pi = c % 2
                        ys = []
                        for h2 in range(2):
                            if h2 == 0:
                                slot = wload(s_wup[l, 2 * c:2 * c + 2].rearrange("a p n -> p a n"), 2048)
                                slot = slot.rearrange("p (a k n) -> p a k n", a=2, k=8)
                            pp = bank("P")
                            for kc in range(8):
                                mm(pp[:], slot[:, h2, kc, :], hT[:, kc, :], start=(kc == 0), stop=(kc == 7))
                            rw = fraw[pi][h2]
                            jj = 2 * c + h2
                            cp("pool", rw[:, 0:2], halo_f[l][:, jj, :])
                            cp("act", rw[:, 2:514], pp[:])
                            y = fy[pi][h2]
                            act(y, pp[:], AF.Copy, scale=cwf[l][:, jj, 2:3])
                            cp("pool", halo_f[l][:, jj, :], rw[:, 512:514])
                            stt("dve", y, rw[:, 0:512], cwf[l][:, jj, 0:1], y, ALU.mult, ALU.add)
                            stt("dve", y, rw[:, 1:513], cwf[l][:, jj, 1:2], y, ALU.mult, ALU.add)
                            ys.append(y)
                        act(fsil[pi], ys[0], AF.Silu)
                        tt("dve", actT[:, c, :], fsil[pi], ys[1], ALU.mult)
                    for d in range(8):
                        slot = wload(s_wdn[l, d], 22 * 128).rearrange("p (k n) -> p k n", k=22)
                        pp = bank("P")
                        for kc in range(22):
                            mm(pp[:], slot[:, kc, :], actT[:, kc, :], start=(kc == 0), stop=(kc == 21))
                        tt("dve", xT[:, d, :], xT[:, d, :], pp[:], ALU.add)

                    _chk("P7")
                rmsnorm_to_hT(sq2, rstd2, inv1024)
                tt("dve", xT[:], xT[:], bc(rstd2.unsqueeze(1), [128, 8, 512]), ALU.mult)
                tt("dve", xT[:], xT[:], bc(gfin[:].unsqueeze(2), [128, 8, 512]), ALU.mult)
                for tl in range(4):
                    for half in range(2):
                        pt = bank("M")
                        for k4 in range(4):
                            kc = 4 * half + k4
                            trp(pt[:, 128 * k4:128 * k4 + 128], xT[:, kc, 128 * tl:128 * tl + 128], identf[:])
                        cp("act" if half == 0 else "dve", xst_out[:, 512 * half:512 * half + 512], pt[:])
                    S.dma("pool", out_d[sq_i, t0 + 128 * tl:t0 + 128 * tl + 128, :], xst_out)

        S.emit()
    return consts


_CACHE = {}


def kernel(**inputs):
    x = np.ascontiguousarray(inputs["x"], dtype=np.float32)
    B, SL, _ = x.shape
    NCORE = 8
    NSEQ = B // NCORE
    key = (SL, NSEQ)
    if key not in _CACHE:
        nc = bass.Bass("TRN2", target_bir_lowering=False)
        consts = build(nc, SL, NSEQ, 2)
        _CACHE[key] = (nc, consts)
    nc, consts = _CACHE[key]
    in_maps = []
    for c in range(NCORE):
        m = {"x": x[c * NSEQ:(c + 1) * NSEQ]}
        for n in W_NAMES:
            m[n] = np.ascontiguousarray(inputs[n], dtype=np.float32)
        m.update(consts)
        in_maps.append(m)
    res = run_bass_kernel_spmd(nc, in_maps, core_ids=list(range(NCORE)))
    out = np.concatenate([np.asarray(r["out"]) for r in res.results], axis=0)
    return out.astype(np.float32)
```
